# Optimizing a Trainium2 kernel written in Bass

```python
import math
import jax, jax.numpy as jnp
from jax import lax
import numpy as np

D_MODEL = 1024
BATCH = 4
SEQ = 8192
DEPTH = 4

CTX_LEN = 256
GRID_W = 64
HEAD_DIM = 64
ATTN_SCALE = HEAD_DIM ** -0.5
ROPE_THETA = 10000.0
NEG_INF = -1e30
Q_BLOCK = 128

NA_HEADS = 8
NA_KH = 8
NA_KW = 16
SWA_Q_HEADS = 8
SWA_KV_HEADS = 2
SWA_WINDOW = 128
GA_Q_HEADS = 8
GA_KV_HEADS = 2
SSM_HEADS = 8
SSM_HEAD_DIM = 64
SSM_INNER = SSM_HEADS * SSM_HEAD_DIM
SSM_GROUPS = 2
SSM_STATE = 128
SSM_CONV = 5
SSM_CONV_CH = SSM_INNER + 2 * SSM_GROUPS * SSM_STATE
SSM_CHUNK = 128
N_BRANCH = 4
BRANCH_W = 512
D_FF = 2816
N_EXPERTS = 8
TOP_K = 2
D_FF_EXPERT = 3584
MOE_BLOCK = 512
ALPHA = (2 * DEPTH) ** 0.25
BETA = (8 * DEPTH) ** -0.25
LN_EPS = 1e-5
RMS_EPS = 1e-6

IN_SPLITS = (
    NA_HEADS * HEAD_DIM, NA_HEADS * HEAD_DIM, NA_HEADS * HEAD_DIM,
    SWA_Q_HEADS * HEAD_DIM, SWA_KV_HEADS * HEAD_DIM, SWA_KV_HEADS * HEAD_DIM,
    GA_Q_HEADS * HEAD_DIM, GA_KV_HEADS * HEAD_DIM, GA_KV_HEADS * HEAD_DIM,
    SSM_INNER, SSM_CONV_CH, 2 * SSM_HEADS,
)
D_IN = sum(IN_SPLITS)

kernel_name = 'hybrid_gated_na_swa_ga_ssd_moe_dit'


def layer_norm(x, g, b):
    xf = x.astype(jnp.float32)
    mu = jnp.mean(xf, -1, keepdims=True)
    var = jnp.mean(jnp.square(xf - mu), -1, keepdims=True)
    y = (xf - mu) * lax.rsqrt(var + LN_EPS) * g.astype(jnp.float32) + b.astype(jnp.float32)
    return y.astype(x.dtype)


def rms_norm(x, g):
    xf = x.astype(jnp.float32)
    y = xf * lax.rsqrt(jnp.mean(jnp.square(xf), -1, keepdims=True) + RMS_EPS) * g.astype(jnp.float32)
    return y.astype(x.dtype)


def heads(t):
    return t.reshape(t.shape[0], t.shape[1], -1, HEAD_DIM)


def group(t, n_kv):
    return t.reshape(t.shape[0], t.shape[1], n_kv, -1, HEAD_DIM)


def split_cols(p):
    return jnp.split(p, np.cumsum(IN_SPLITS)[:-1].tolist(), axis=-1)


def axial_rope_tables(n_tok):
    t = jnp.arange(n_tok, dtype=jnp.int32)
    row = (t // GRID_W).astype(jnp.float32)
    col = (t % GRID_W).astype(jnp.float32)
    n_freq = HEAD_DIM // 4
    inv_freq = ROPE_THETA ** (-jnp.arange(n_freq, dtype=jnp.float32) / n_freq)
    ang = jnp.concatenate([row[:, None] * inv_freq, col[:, None] * inv_freq], axis=-1)
    return jnp.cos(ang), jnp.sin(ang)


def apply_rope(x, cos, sin):
    half = HEAD_DIM // 2
    x1, x2 = x[..., :half], x[..., half:]
    cs = cos[:, None, :].astype(x.dtype)
    sn = sin[:, None, :].astype(x.dtype)
    return jnp.concatenate([x1 * cs - x2 * sn, x2 * cs + x1 * sn], axis=-1)


def attend(q, k, v, mask=None, sink=None):
    s = jnp.einsum('bqkgd,bjkd->bkgqj', q, k).astype(jnp.float32) * ATTN_SCALE
    if mask is not None:
        s = jnp.where(mask, s, NEG_INF)
    if sink is not None:
        s_sink = jnp.broadcast_to(sink.astype(jnp.float32)[None, :, :, None, None], s.shape[:-1] + (1,))
        s = jnp.concatenate([s, s_sink], axis=-1)
    p = jax.nn.softmax(s, axis=-1)
    if sink is not None:
        p = p[..., :-1]
    return jnp.einsum('bkgqj,bjkd->bqkgd', p.astype(v.dtype), v)


def neighbourhood_attention(q, k, v, qc, kc, vc, rpb, with_ctx):
    bsz, n_tok, n_h, dh = q.shape
    rows = n_tok // GRID_W
    kh = min(NA_KH, rows)
    qg = q.reshape(bsz, rows, GRID_W, n_h, dh)
    kg = k.reshape(bsz, rows, GRID_W, n_h, dh)
    vg = v.reshape(bsz, rows, GRID_W, n_h, dh)
    r_idx = jnp.arange(rows, dtype=jnp.int32)
    row_start = jnp.clip(r_idx - kh // 2, 0, rows - kh)
    c_idx = jnp.arange(GRID_W, dtype=jnp.int32)
    col_start = jnp.clip(c_idx - NA_KW // 2, 0, GRID_W - NA_KW)
    col_idx = col_start[:, None] + jnp.arange(NA_KW, dtype=jnp.int32)
    dc = col_idx - c_idx[:, None] + NA_KW - 1
    n_loc = kh * NA_KW

    def row_fn(args):
        r, start, q_row = args
        kb = lax.dynamic_slice_in_dim(kg, start, kh, axis=1)[:, :, col_idx]
        vb = lax.dynamic_slice_in_dim(vg, start, kh, axis=1)[:, :, col_idx]
        dr = start + jnp.arange(kh, dtype=jnp.int32) - r + NA_KH - 1
        bias = rpb[:, dr][:, :, dc].transpose(0, 2, 1, 3).astype(jnp.float32)
        s_loc = jnp.einsum('bqhd,bkqwhd->bhqkw', q_row, kb).astype(jnp.float32) * ATTN_SCALE + bias[None]
        s_ctx = jnp.einsum('bqhd,bjhd->bhqj', q_row, kc).astype(jnp.float32) * ATTN_SCALE
        s = jnp.concatenate([s_loc.reshape(bsz, n_h, GRID_W, n_loc), s_ctx], axis=-1)
        p = jax.nn.softmax(s, axis=-1).astype(v.dtype)
        p_loc = p[..., :n_loc].reshape(bsz, n_h, GRID_W, kh, NA_KW)
        return (jnp.einsum('bhqkw,bkqwhd->bqhd', p_loc, vb)
                + jnp.einsum('bhqj,bjhd->bqhd', p[..., n_loc:], vc))

    out = lax.map(row_fn, (r_idx, row_start, qg.transpose(1, 0, 2, 3, 4)))
    out = out.transpose(1, 0, 2, 3, 4).reshape(bsz, n_tok, n_h * dh)
    out_c = attend(qc[:, :, :, None], kc, vc).reshape(bsz, qc.shape[1], -1) if with_ctx else None
    return out, out_c


def window_attention(q, k, v, qc, kc, vc, sink, with_ctx):
    bsz, n_tok = q.shape[:2]
    n_blk = n_tok // Q_BLOCK
    band = Q_BLOCK + 2 * SWA_WINDOW
    kp = jnp.pad(k, ((0, 0), (SWA_WINDOW, SWA_WINDOW), (0, 0), (0, 0)))
    vp = jnp.pad(v, ((0, 0), (SWA_WINDOW, SWA_WINDOW), (0, 0), (0, 0)))
    qi = jnp.arange(Q_BLOCK, dtype=jnp.int32)
    kj = jnp.arange(band, dtype=jnp.int32)
    in_win = jnp.abs(kj[None, :] - SWA_WINDOW - qi[:, None]) <= SWA_WINDOW
    ctx_ok = jnp.ones((Q_BLOCK, kc.shape[1]), dtype=bool)

    def block(args):
        n, qb = args
        kb = lax.dynamic_slice_in_dim(kp, n * Q_BLOCK, band, axis=1)
        vb = lax.dynamic_slice_in_dim(vp, n * Q_BLOCK, band, axis=1)
        key_pos = n * Q_BLOCK - SWA_WINDOW + kj
        valid = in_win & ((key_pos >= 0) & (key_pos < n_tok))[None, :]
        mask = jnp.concatenate([valid, ctx_ok], axis=-1)
        return attend(qb, jnp.concatenate([kb, kc], axis=1), jnp.concatenate([vb, vc], axis=1), mask, sink)

    qbl = q.reshape(bsz, n_blk, Q_BLOCK, *q.shape[2:]).swapaxes(0, 1)
    out = lax.map(block, (jnp.arange(n_blk, dtype=jnp.int32), qbl)).swapaxes(0, 1).reshape(bsz, n_tok, -1)
    out_c = attend(qc, kc, vc, sink=sink).reshape(bsz, qc.shape[1], -1) if with_ctx else None
    return out, out_c


def global_attention(q, k, v, qc, kc, vc, with_ctx):
    bsz, n_tok = q.shape[:2]
    n_blk = n_tok // Q_BLOCK
    k_all = jnp.concatenate([k, kc], axis=1)
    v_all = jnp.concatenate([v, vc], axis=1)
    qbl = q.reshape(bsz, n_blk, Q_BLOCK, *q.shape[2:]).swapaxes(0, 1)
    out = lax.map(lambda qb: attend(qb, k_all, v_all), qbl).swapaxes(0, 1).reshape(bsz, n_tok, -1)
    out_c = attend(qc, kc, vc).reshape(bsz, qc.shape[1], -1) if with_ctx else None
    return out, out_c


def dwconv_silu(u, w, b):
    ch = u.shape[-1]
    y = lax.conv_general_dilated(u, w[:, None, :].astype(u.dtype), window_strides=(1,),
                                 padding=[(SSM_CONV // 2, SSM_CONV // 2)],
                                 dimension_numbers=('NWC', 'WIO', 'NWC'), feature_group_count=ch)
    return jax.nn.silu(y + b)


def segsum_exp(a):
    q_len = a.shape[-1]
    cs = jnp.cumsum(a, axis=-1)
    diff = cs[..., :, None] - cs[..., None, :]
    mask = jnp.tril(jnp.ones((q_len, q_len), dtype=bool))
    return jnp.where(mask, jnp.exp(jnp.where(mask, diff, 0.0)), 0.0)


def ssd_scan(x, dt, a, bm, cm, h0, return_y):
    bsz, n_tok, n_h, p_dim = x.shape
    n_st = bm.shape[-1]
    n_c = n_tok // SSM_CHUNK
    xdt = (x.astype(jnp.float32) * dt[..., None]).reshape(bsz, n_c, SSM_CHUNK, n_h, p_dim)
    da = (dt * a.astype(jnp.float32)).reshape(bsz, n_c, SSM_CHUNK, n_h).transpose(0, 3, 1, 2)
    a_cum = jnp.cumsum(da, axis=-1)
    bc = bm.astype(jnp.float32).reshape(bsz, n_c, SSM_CHUNK, n_h, n_st)
    cc = cm.astype(jnp.float32).reshape(bsz, n_c, SSM_CHUNK, n_h, n_st)
    decay_states = jnp.exp(a_cum[..., -1:] - a_cum)
    states = jnp.einsum('bclhn,bhcl,bclhp->bchpn', bc, decay_states, xdt)
    chunk_decay = jnp.exp(a_cum[..., -1])

    def step(h, inp):
        st, dec = inp
        return h * dec[..., None, None] + st, (h if return_y else None)

    h_final, h_in = lax.scan(step, h0, (states.transpose(1, 0, 2, 3, 4), chunk_decay.transpose(2, 0, 1)))
    if not return_y:
        return None, h_final
    h_in = h_in.transpose(1, 0, 2, 3, 4)
    y_diag = jnp.einsum('bclhn,bcshn,bhcls,bcshp->bclhp', cc, bc, segsum_exp(da), xdt)
    y_off = jnp.einsum('bclhn,bchpn,bhcl->bclhp', cc, h_in, jnp.exp(a_cum))
    return (y_diag + y_off).reshape(bsz, n_tok, n_h, p_dim), h_final


def mamba_branch(z, xbc, dt_raw, zc, xbcc, dtc_raw, conv_w, conv_b, dt_bias, a_log, d_skip, norm_g, with_ctx):
    a = -jnp.exp(a_log.astype(jnp.float32))
    rep = SSM_HEADS // SSM_GROUPS
    gn = SSM_GROUPS * SSM_STATE

    def prep(xbc_, dt_raw_):
        u = dwconv_silu(xbc_, conv_w, conv_b)
        bsz, n_tok = u.shape[:2]
        xs = u[..., :SSM_INNER].reshape(bsz, n_tok, SSM_HEADS, SSM_HEAD_DIM)
        bm = jnp.repeat(u[..., SSM_INNER:SSM_INNER + gn].reshape(bsz, n_tok, SSM_GROUPS, SSM_STATE), rep, axis=2)
        cm = jnp.repeat(u[..., SSM_INNER + gn:].reshape(bsz, n_tok, SSM_GROUPS, SSM_STATE), rep, axis=2)
        dt = jax.nn.softplus(dt_raw_.astype(jnp.float32).reshape(bsz, n_tok, 2, SSM_HEADS) + dt_bias.astype(jnp.float32))
        return xs, bm, cm, dt

    xl, bl, cl, dtl = prep(xbc, dt_raw)
    xc, bc, cc, dtc = prep(xbcc, dtc_raw)
    fl = lambda t: jnp.flip(t, axis=1)
    h0 = jnp.zeros((xl.shape[0], SSM_HEADS, SSM_HEAD_DIM, SSM_STATE), jnp.float32)
    yc_f, hc_f = ssd_scan(xc, dtc[:, :, 0], a[0], bc, cc, h0, with_ctx)
    yc_b, hc_b = ssd_scan(fl(xc), fl(dtc[:, :, 1]), a[1], fl(bc), fl(cc), h0, with_ctx)
    yl_f, _ = ssd_scan(xl, dtl[:, :, 0], a[0], bl, cl, hc_f, True)
    yl_b, _ = ssd_scan(fl(xl), fl(dtl[:, :, 1]), a[1], fl(bl), fl(cl), hc_b, True)

    def finish(y, xs, z_):
        y = y + d_skip.astype(jnp.float32)[:, None] * xs.astype(jnp.float32)
        y = y.reshape(y.shape[0], y.shape[1], SSM_INNER)
        return rms_norm(y * jax.nn.silu(z_.astype(jnp.float32)), norm_g).astype(z_.dtype)

    out = finish(yl_f + fl(yl_b), xl, z)
    out_c = finish(yc_f + fl(yc_b), xc, zc) if with_ctx else None
    return out, out_c


def hybrid_mixer(h, hc, w_in, w_gate, b_gate, na_rpb, swa_sink, qk_gain_q, qk_gain_k, conv_w, conv_b,
                 dt_bias, a_log, d_skip, ssm_norm_g, w_branch, w_out, with_ctx):
    cos, sin = axial_rope_tables(h.shape[1])
    rope = lambda t: apply_rope(t, cos, sin)
    (na_q, na_k, na_v, sw_q, sw_k, sw_v, ga_q, ga_k, ga_v, z, xbc, dt_raw) = split_cols(h @ w_in)
    (na_qc, na_kc, na_vc, sw_qc, sw_kc, sw_vc, ga_qc, ga_kc, ga_vc, zc, xbcc, dtc_raw) = split_cols(hc @ w_in)
    o_a, o_ac = neighbourhood_attention(heads(na_q), heads(na_k), heads(na_v),
                                        heads(na_qc), heads(na_kc), heads(na_vc), na_rpb, with_ctx)
    o_b, o_bc = window_attention(group(rope(heads(sw_q)), SWA_KV_HEADS), rope(heads(sw_k)), heads(sw_v),
                                 group(heads(sw_qc), SWA_KV_HEADS), heads(sw_kc), heads(sw_vc),
                                 swa_sink.reshape(SWA_KV_HEADS, -1), with_ctx)
    o_c, o_cc = global_attention(group(rope(rms_norm(heads(ga_q), qk_gain_q)), GA_KV_HEADS),
                                 rope(rms_norm(heads(ga_k), qk_gain_k)), heads(ga_v),
                                 group(rms_norm(heads(ga_qc), qk_gain_q), GA_KV_HEADS),
                                 rms_norm(heads(ga_kc), qk_gain_k), heads(ga_vc), with_ctx)
    o_d, o_dc = mamba_branch(z, xbc, dt_raw, zc, xbcc, dtc_raw, conv_w, conv_b, dt_bias, a_log, d_skip,
                             ssm_norm_g, with_ctx)

    def merge(h_in, branches):
        terms = [jax.nn.sigmoid(h_in @ w_gate[i] + b_gate[i]) * (u @ w_branch[i]) for i, u in enumerate(branches)]
        return (terms[0] + terms[1] + terms[2] + terms[3]) @ w_out

    out = merge(h, (o_a, o_b, o_c, o_d))
    out_c = merge(hc, (o_ac, o_bc, o_cc, o_dc)) if with_ctx else None
    return out, out_c


def swiglu(h, w_up, w_down):
    g, u = jnp.split(h @ w_up, 2, axis=-1)
    return (jax.nn.silu(g) * u) @ w_down


def moe_swiglu(tokens, w_router, b_router, w_up, w_down):
    n_tok, d = tokens.shape
    logits = (tokens @ w_router).astype(jnp.float32) + b_router.astype(jnp.float32)
    top_logit, top_idx = lax.top_k(logits, TOP_K)
    gate = jax.nn.softmax(top_logit, axis=-1)
    n_slot = n_tok * TOP_K
    expert = top_idx.reshape(-1)
    token = jnp.repeat(jnp.arange(n_tok, dtype=jnp.int32), TOP_K)
    order = jnp.argsort(expert)
    e_s, t_s, w_s = expert[order], token[order], gate.reshape(-1)[order]
    counts = jax.ops.segment_sum(jnp.ones_like(expert), expert, num_segments=N_EXPERTS)
    padded = (counts + MOE_BLOCK - 1) // MOE_BLOCK * MOE_BLOCK
    grp_start = jnp.cumsum(counts) - counts
    pad_end = jnp.cumsum(padded)
    pad_start = pad_end - padded
    dest = pad_start[e_s] + jnp.arange(n_slot, dtype=jnp.int32) - grp_start[e_s]
    n_blk = -(-n_slot // MOE_BLOCK) + N_EXPERTS
    x_pad = jnp.zeros((n_blk * MOE_BLOCK, d), tokens.dtype).at[dest].set(tokens[t_s])
    blk_expert = jnp.minimum(
        jnp.searchsorted(pad_end, jnp.arange(n_blk, dtype=jnp.int32) * MOE_BLOCK, side='right'), N_EXPERTS - 1)

    def expert_block(args):
        xb, e = args
        return swiglu(xb, w_up[e], w_down[e])

    y_pad = lax.map(expert_block, (x_pad.reshape(n_blk, MOE_BLOCK, d), blk_expert)).reshape(-1, d)
    y = y_pad[dest] * w_s[:, None].astype(tokens.dtype)
    return jnp.zeros_like(tokens).at[t_s].add(y)


def setup_inputs(seed: int = 0) -> dict:
    key = jax.random.key(seed)
    ks = iter(jax.random.split(key, 48))
    nrm = lambda shape, scale: jax.random.normal(next(ks), shape, jnp.float32) * scale
    d = D_MODEL
    n_l = DEPTH
    n_dense = (DEPTH + 1) // 2
    n_moe = DEPTH // 2
    x = nrm((BATCH, SEQ, d), 1.0)
    c = nrm((BATCH, d), 1.0)
    ctx = nrm((BATCH, CTX_LEN, d), 1.0)
    c_ctx = nrm((d,), 1.0)
    w_mod = nrm((n_l, d, 6 * d), d ** -0.5)
    b_mod = nrm((n_l, 6 * d), 0.02)
    w_in = nrm((n_l, d, D_IN), d ** -0.5)
    w_gate = nrm((n_l, N_BRANCH, d, d), d ** -0.5)
    b_gate = nrm((n_l, N_BRANCH, d), 0.02)
    na_rpb = nrm((n_l, NA_HEADS, 2 * NA_KH - 1, 2 * NA_KW - 1), 0.5)
    swa_sink = nrm((n_l, SWA_Q_HEADS), 1.0)
    qk_gain_q = 1.0 + nrm((n_l, HEAD_DIM), 0.02)
    qk_gain_k = 1.0 + nrm((n_l, HEAD_DIM), 0.02)
    conv_w = nrm((n_l, SSM_CONV, SSM_CONV_CH), SSM_CONV ** -0.5)
    conv_b = nrm((n_l, SSM_CONV_CH), 0.02)
    dt0 = jnp.exp(jax.random.uniform(next(ks), (n_l, 2, SSM_HEADS), jnp.float32,
                                     minval=math.log(1e-3), maxval=math.log(1e-1)))
    dt_bias = dt0 + jnp.log(-jnp.expm1(-dt0))
    a_log = jnp.log(jax.random.uniform(next(ks), (n_l, 2, SSM_HEADS), jnp.float32, minval=1.0, maxval=16.0))
    d_skip = 1.0 + nrm((n_l, SSM_HEADS), 0.1)
    ssm_norm_g = 1.0 + nrm((n_l, SSM_INNER), 0.02)
    w_branch = nrm((n_l, N_BRANCH, BRANCH_W, d), BRANCH_W ** -0.5)
    w_out = nrm((n_l, d, d), d ** -0.5 * BETA)
    ln1_g = 1.0 + nrm((n_l, d), 0.02)
    ln1_b = nrm((n_l, d), 0.02)
    ln2_g = 1.0 + nrm((n_l, d), 0.02)
    ln2_b = nrm((n_l, d), 0.02)
    ffn_w_up = nrm((n_dense, d, 2 * D_FF), d ** -0.5)
    ffn_w_down = nrm((n_dense, D_FF, d), D_FF ** -0.5 * BETA)
    moe_w_router = nrm((n_moe, d, N_EXPERTS), d ** -0.5)
    moe_b_router = nrm((n_moe, N_EXPERTS), 0.01)
    moe_w_up = nrm((n_moe, N_EXPERTS, d, 2 * D_FF_EXPERT), d ** -0.5)
    moe_w_down = nrm((n_moe, N_EXPERTS, D_FF_EXPERT, d), D_FF_EXPERT ** -0.5 * BETA)
    return {'x': x, 'c': c, 'ctx': ctx, 'c_ctx': c_ctx, 'w_mod': w_mod, 'b_mod': b_mod, 'w_in': w_in,
            'w_gate': w_gate, 'b_gate': b_gate, 'na_rpb': na_rpb, 'swa_sink': swa_sink,
            'qk_gain_q': qk_gain_q, 'qk_gain_k': qk_gain_k, 'conv_w': conv_w, 'conv_b': conv_b,
            'dt_bias': dt_bias, 'a_log': a_log, 'd_skip': d_skip, 'ssm_norm_g': ssm_norm_g,
            'w_branch': w_branch, 'w_out': w_out, 'ln1_g': ln1_g, 'ln1_b': ln1_b, 'ln2_g': ln2_g,
            'ln2_b': ln2_b, 'ffn_w_up': ffn_w_up, 'ffn_w_down': ffn_w_down, 'moe_w_router': moe_w_router,
            'moe_b_router': moe_b_router, 'moe_w_up': moe_w_up, 'moe_w_down': moe_w_down}


def reference(x, c, ctx, c_ctx, w_mod, b_mod, w_in, w_gate, b_gate, na_rpb, swa_sink, qk_gain_q, qk_gain_k,
              conv_w, conv_b, dt_bias, a_log, d_skip, ssm_norm_g, w_branch, w_out, ln1_g, ln1_b, ln2_g, ln2_b,
              ffn_w_up, ffn_w_down, moe_w_router, moe_b_router, moe_w_up, moe_w_down):
    s_lat = jax.nn.silu(c)
    s_ctx = jax.nn.silu(c_ctx)
    for l in range(DEPTH):
        with_ctx = l < DEPTH - 1
        m = jnp.split((s_lat @ w_mod[l] + b_mod[l])[:, None, :], 6, axis=-1)
        mc = jnp.split(s_ctx @ w_mod[l] + b_mod[l], 6, axis=-1)
        h = x * (1 + m[1]) + m[0]
        hc = ctx * (1 + mc[1]) + mc[0]
        o, oc = hybrid_mixer(h, hc, w_in[l], w_gate[l], b_gate[l], na_rpb[l], swa_sink[l], qk_gain_q[l],
                             qk_gain_k[l], conv_w[l], conv_b[l], dt_bias[l], a_log[l], d_skip[l],
                             ssm_norm_g[l], w_branch[l], w_out[l], with_ctx)
        x = layer_norm(ALPHA * x + m[2] * o, ln1_g[l], ln1_b[l])
        h2 = x * (1 + m[4]) + m[3]
        if with_ctx:
            ctx = layer_norm(ALPHA * ctx + mc[2] * oc, ln1_g[l], ln1_b[l])
            h2c = ctx * (1 + mc[4]) + mc[3]
        i = l // 2
        if l % 2 == 0:
            f = swiglu(h2, ffn_w_up[i], ffn_w_down[i])
            if with_ctx:
                fc = swiglu(h2c, ffn_w_up[i], ffn_w_down[i])
        elif with_ctx:
            n_lat = h2.shape[0] * h2.shape[1]
            tok = jnp.concatenate([h2.reshape(-1, D_MODEL), h2c.reshape(-1, D_MODEL)], axis=0)
            y = moe_swiglu(tok, moe_w_router[i], moe_b_router[i], moe_w_up[i], moe_w_down[i])
            f = y[:n_lat].reshape(h2.shape)
            fc = y[n_lat:].reshape(h2c.shape)
        else:
            f = moe_swiglu(h2.reshape(-1, D_MODEL), moe_w_router[i], moe_b_router[i], moe_w_up[i],
                           moe_w_down[i]).reshape(h2.shape)
        x = layer_norm(ALPHA * x + m[5] * f, ln2_g[l], ln2_b[l])
        if with_ctx:
            ctx = layer_norm(ALPHA * ctx + mc[5] * fc, ln2_g[l], ln2_b[l])
    return x
```

```python
import contextlib
import numpy as np
import ml_dtypes
import concourse.bass as bass
import concourse.mybir as mybir
from concourse.bass_utils import run_bass_kernel_spmd

F32 = mybir.dt.float32
BF16 = mybir.dt.bfloat16
AF = mybir.ActivationFunctionType
ALU = mybir.AluOpType

D = 1024
S_LAT = 8192
S_CTX = 256
T = S_LAT + S_CTX
NT = T // 128
DEPTH = 4
ALPHA = (2 * DEPTH) ** 0.25
SCALE = 64 ** -0.5
LN_EPS = 1e-5
RMS_EPS = 1e-6
D_FF = 2816
D_FFE = 3584
NE = 8


class Buf:
    __slots__ = ("t", "lw", "rd", "name")

    def __init__(self, t, name=""):
        self.t = t
        self.lw = None
        self.rd = {}
        self.name = name

    def __getitem__(self, idx):
        return self.t[idx]


class P:
    def __init__(self, nc, n_dma_sems=12):
        self.nc = nc
        self.eng = {"pe": nc.tensor, "act": nc.scalar, "dve": nc.vector, "pool": nc.gpsimd, "sp": nc.sync}
        self.sems = {}
        self.cnt = {}
        for k in ("pe", "act", "dve", "pool"):
            self.sems[k] = nc.alloc_semaphore(name="s_" + k)
            self.cnt[k] = 0
        self.nd = n_dma_sems
        for i in range(n_dma_sems):
            self.sems[("d", i)] = nc.alloc_semaphore(name="s_d%d" % i)
            self.cnt[("d", i)] = 0
        self.dnext = 0
        self.seen = {e: {} for e in self.eng}
        self.nins = 0
        self.rots = {}
        self.uid = 0
        self.stack = contextlib.ExitStack()

    def sb(self, name, shape, dt):
        self.uid += 1
        name = "sb%d_%s" % (self.uid, name)
        return Buf(self.stack.enter_context(self.nc.sbuf_tensor(name, list(shape), dt)), name)

    def ps(self, name, shape, dt=F32):
        self.uid += 1
        name = "ps%d_%s" % (self.uid, name)
        return Buf(self.stack.enter_context(self.nc.psum_tensor(name, list(shape), dt)), name)

    def rot(self, key, bufs):
        self.rots[key] = [list(bufs), 0]

    def nxt(self, key):
        r = self.rots[key]
        b = r[0][r[1] % len(r[0])]
        r[1] += 1
        return b

    def _wait(self, e, key, val):
        if val <= 0 or self.seen[e].get(key, 0) >= val:
            return
        self.eng[e].wait_ge(self.sems[key], val)
        self.seen[e][key] = val

    def _deps(self, e, r, w):
        for b in r:
            if b.lw is not None and not (e == "pe" and b.lw[0] == "pe"):
                self._wait(e, *b.lw)
        for b in w:
            if b.lw is not None and not (e == "pe" and b.lw[0] == "pe"):
                self._wait(e, *b.lw)
            for k, v in b.rd.items():
                if not (e == "pe" and k == "pe"):
                    self._wait(e, k, v)

    def op(self, e, fn, r=(), w=()):
        self._deps(e, r, w)
        ins = fn()
        self.cnt[e] += 1
        c = self.cnt[e]
        ins.then_inc(self.sems[e], 1)
        self.nins += 1
        for b in r:
            b.rd[e] = c
        for b in w:
            b.lw = (e, c)
            b.rd = {}
        return ins

    def dma(self, out_ap, in_ap, r=(), w=(), q="sp"):
        i = self.dnext
        self.dnext = (self.dnext + 1) % self.nd
        key = ("d", i)
        self._wait(q, key, self.cnt[key])
        self._deps(q, r, w)
        ins = self.eng[q].dma_start(out=out_ap, in_=in_ap)
        self.cnt[key] += 16
        c = self.cnt[key]
        ins.then_inc(self.sems[key], 16)
        self.nins += 1
        for b in r:
            b.rd[key] = c
        for b in w:
            b.lw = (key, c)
            b.rd = {}
        return ins

    def barrier(self):
        for e in self.eng:
            for k in self.cnt:
                self._wait(e, k, self.cnt[k])

    def finish(self):
        self.barrier()

    @contextlib.contextmanager
    def scope(self):
        old = self.stack
        with contextlib.ExitStack() as st:
            self.stack = st
            yield
            self.barrier()
        self.stack = old

    def mm(self, ob, o, lb, l, rb, r, start=True, stop=True):
        nc = self.nc
        return self.op("pe", lambda: nc.tensor.matmul(o, l, r, start=start, stop=stop), r=[lb, rb], w=[ob])

    def tr(self, ob, o, ib, i, idb, idn):
        nc = self.nc
        return self.op("pe", lambda: nc.tensor.transpose(o, i, idn), r=[ib, idb], w=[ob])

    def act(self, ob, o, ib, i, func, bias=None, scale=None, extra_r=()):
        nc = self.nc
        kw = {}
        if bias is not None:
            kw["bias"] = bias
        if scale is not None:
            kw["scale"] = scale
        return self.op("act", lambda: nc.scalar.activation(out=o, in_=i, func=func, **kw), r=[ib] + list(extra_r), w=[ob])

    def tt(self, e, ob, o, ab, a, bb, b, op):
        en = self.eng[e]
        return self.op(e, lambda: en.tensor_tensor(out=o, in0=a, in1=b, op=op), r=[ab, bb], w=[ob])

    def ts(self, e, ob, o, ab, a, s1, s2, op0, op1=None, extra_r=()):
        en = self.eng[e]
        if op1 is None:
            return self.op(e, lambda: en.tensor_scalar(out=o, in0=a, scalar1=s1, scalar2=None, op0=op0),
                           r=[ab] + list(extra_r), w=[ob])
        return self.op(e, lambda: en.tensor_scalar(out=o, in0=a, scalar1=s1, scalar2=s2, op0=op0, op1=op1),
                       r=[ab] + list(extra_r), w=[ob])

    def stt(self, e, ob, o, ab, a, s, bb, b, op0, op1, extra_r=()):
        e = "dve"
        en = self.eng[e]
        return self.op(e, lambda: en.scalar_tensor_tensor(out=o, in0=a, scalar=s, in1=b, op0=op0, op1=op1),
                       r=[ab, bb] + list(extra_r), w=[ob])

    def cp(self, e, ob, o, ib, i):
        if e == "act":
            nc = self.nc
            return self.op("act", lambda: nc.scalar.copy(out=o, in_=i), r=[ib], w=[ob])
        en = self.eng[e]
        return self.op(e, lambda: en.tensor_copy(out=o, in_=i), r=[ib], w=[ob])

    def memset(self, e, ob, o, v):
        en = self.eng[e]
        return self.op(e, lambda: en.memset(o, v), w=[ob])


DR = Buf(None, "dram_ro")


def new_nc():
    return bass.Bass("TRN2", target_bir_lowering=False)


def din(nc, name, shape, dt=F32):
    return nc.dram_tensor(name, list(shape), dt, kind="ExternalInput").ap()


def dout(nc, name, shape, dt=F32):
    return nc.dram_tensor(name, list(shape), dt, kind="ExternalOutput").ap()


def emit_modulation(p, nc, cvec, wmod, bmod, nj, psb, name):
    cv = p.sb(name + "_cv", [128, 8, 2], F32)
    sv = p.sb(name + "_sv", [128, 8, 2], BF16)
    bm = p.sb(name + "_bm", [128, nj], F32)
    mod = p.sb(name + "_mod", [128, nj, 2], F32)
    wst = [p.sb(name + "_w%d" % i, [128, 8, 512], BF16) for i in range(2)]
    p.dma(cv[:], cvec, w=[cv])
    p.dma(bm[:], bmod, w=[bm])
    p.act(sv, sv[:], cv, cv[:], AF.Silu)
    wv = wmod.rearrange("(kc p) n -> p kc n", p=128)
    for g in range(nj // 4):
        wb = wst[g % 2]
        p.dma(wb[:], wv[:, :, g * 512:(g + 1) * 512], w=[wb], q="pool")
        for jj in range(4):
            j = g * 4 + jj
            ps = psb[j % len(psb)]
            for kc in range(8):
                p.mm(ps, ps[:, 0:2], wb, wb[:, kc, jj * 128:(jj + 1) * 128], sv, sv[:, kc, :], start=(kc == 0), stop=(kc == 7))
            p.ts("dve", mod, mod[:, j, :], ps, ps[:, 0:2], bm[:, j:j + 1], None, ALU.add, extra_r=[bm])
    return mod


def emit_attention(p, nc, QT, q_ap, C, blocks, onesb, out_buf, out_ap, odd, extra_den, tmpo):
    On = p.nxt("psN")
    Od = p.nxt("psD")
    n = len(blocks)
    for i, (KT, k_ap, Vb, v_ap, Mb, m_ap) in enumerate(blocks):
        S = p.nxt("psS")
        p.mm(S, S[:, 0:C], KT, k_ap, QT, q_ap)
        Pt = p.nxt("sbP")
        p.act(Pt, Pt[:, 0:C], S, S[:, 0:C], AF.Exp, scale=SCALE)
        if Mb is not None:
            P2 = p.nxt("sbP2")
            p.tt("dve", P2, P2[:, 0:C], Pt, Pt[:, 0:C], Mb, m_ap, ALU.mult)
            Pt = P2
        p.mm(On, On[0:64, 0:C], Vb, v_ap, Pt, Pt[:, 0:C], start=(i == 0), stop=(i == n - 1))
        p.mm(Od, Od[0:64, 0:C], onesb, onesb[:, 0:64], Pt, Pt[:, 0:C], start=(i == 0), stop=(i == n - 1))
    rd = p.nxt("sbR")
    if extra_den is not None:
        eb, e_ap = extra_den
        p.ts("dve", rd, rd[0:64, 0:C], Od, Od[0:64, 0:C], e_ap, None, ALU.add, extra_r=[eb])
        p.op("dve", lambda: nc.vector.reciprocal(out=rd[0:64, 0:C], in_=rd[0:64, 0:C]), r=[rd], w=[rd])
    else:
        p.op("dve", lambda: nc.vector.reciprocal(out=rd[0:64, 0:C], in_=Od[0:64, 0:C]), r=[Od], w=[rd])
    if not odd:
        p.tt("dve", out_buf, out_ap, On, On[0:64, 0:C], rd, rd[0:64, 0:C], ALU.mult)
    else:
        tb = p.nxt(tmpo)
        p.tt("dve", tb, tb[0:64, 0:C], On, On[0:64, 0:C], rd, rd[0:64, 0:C], ALU.mult)
        p.cp("act", out_buf, out_ap, tb, tb[0:64, 0:C])


def tok_chunks(cs=512):
    ch = [(i * cs, cs, False) for i in range(S_LAT // cs)]
    c0 = S_LAT
    while c0 < T:
        c = min(cs, T - c0)
        ch.append((c0, c, True))
        c0 += c
    return ch


def emit_load_h(p, nc, xTv, mod1p, mod, col0, C, is_ctx, lo=0, hi=0):
    xt = p.nxt("xt")
    ht = p.nxt("ht")
    n = C + lo + hi
    p.dma(xt[:, :, 0:n], xTv[:, :, col0 - lo:col0 + C + hi], w=[xt])
    w = 1 if is_ctx else 0
    for kc in range(8):
        e = "dve" if kc % 2 == 0 else "pool"
        p.ts(e, ht, ht[:, kc, 0:n], xt, xt[:, kc, 0:n], mod1p[:, kc, w:w + 1], mod[:, kc, w:w + 1], ALU.mult, ALU.add,
             extra_r=[mod1p, mod])
    return ht, n


def emit_proj(p, ps, ps_ap, W, col0, ncols, ht, n):
    for kc in range(8):
        p.mm(ps, ps_ap, W, W[:, kc, col0:col0 + ncols], ht, ht[:, kc, 0:n], start=(kc == 0), stop=(kc == 7))


def build_M(phases=("ga", "na", "swa", "ssd")):
    nc = new_nc()
    p = P(nc)
    xT = din(nc, "xT", [D, T])
    cvec = din(nc, "cvec", [128, 8, 2])
    wmod = din(nc, "wmod", [D, 2048])
    bmod = din(nc, "bmod", [128, 16])
    w_ga = din(nc, "w_ga", [D, 832])
    w_sw = din(nc, "w_sw", [D, 832])
    w_na = din(nc, "w_na", [D, 768])
    w_sd = din(nc, "w_sd", [D, 776])
    cos2 = din(nc, "cos2", [128, S_LAT])
    sin2 = din(nc, "sin2", [128, S_LAT])
    gains = din(nc, "gains", [128, 4])
    consts = din(nc, "consts", [128, 5, 128])
    identb_d = din(nc, "identb", [128, 128], BF16)
    sinkv = din(nc, "sinkv", [128, 4])
    na_bias = din(nc, "na_bias", [4, 14, 128, 256])
    na_mask = din(nc, "na_mask", [14, 128, 256])
    sw_mask = din(nc, "sw_mask", [4, 128, 256], BF16)
    ssd_small = din(nc, "ssd_small", [128, 32])
    ssd_convw = din(nc, "ssd_convw", [128, 4, 5])
    u_out = dout(nc, "u_out", [3, 2, 128, T], BF16)
    yg_out = dout(nc, "yg_out", [2, 128, T])
    ysc = nc.dram_tensor("ysc", [NT, 128, 256], F32).ap()

    xTv = xT.rearrange("(kc p) t -> p kc t", p=128)

    psb = [p.ps("psb%d" % i, [128, 512], F32) for i in range(7)]
    psbf = p.ps("psbf", [128, 1024], BF16)
    p.rot("psN", psb[0:2])
    p.rot("psD", psb[2:4])
    p.rot("psS", psb[4:7])
    p.rot("psJ", psb)

    cst = p.sb("cst", [128, 5, 128], F32)
    p.dma(cst[:], consts, w=[cst])
    identb = p.sb("identb", [128, 128], BF16)
    p.dma(identb[:], identb_d, w=[identb])
    onesb = p.sb("onesb", [128, 128], BF16)
    p.memset("dve", onesb, onesb[:], 1.0)
    gn = p.sb("gn", [128, 4], F32)
    p.dma(gn[:], gains, w=[gn])

    mod = emit_modulation(p, nc, cvec, wmod, bmod, 16, psb, "mm")
    mod1p = p.sb("mod1p", [128, 8, 2], F32)
    p.ts("dve", mod1p, mod1p[:], mod, mod[:, 8:16, :], 1.0, None, ALU.add)
    p.barrier()

    def attn_phase(kind):
        with p.scope():
            _attn_phase(kind)

    def _attn_phase(kind):
        ga, na, sw = kind == "ga", kind == "na", kind == "swa"
        wsrc = {"ga": w_ga, "swa": w_sw, "na": w_na}[kind]
        ncols = 768 if na else 832
        W = p.sb("W", [128, 8, ncols], BF16)
        p.dma(W[:], wsrc.rearrange("(kc p) n -> p kc n", p=128), w=[W], q="pool")
        QT = [p.sb("QT%d" % i, [128, T], BF16) for i in range(2)]
        nkt = 2 if na else 1
        KT = [p.sb("KT%d" % i, [128, T], BF16) for i in range(nkt)]
        vw = 256 if na else 64
        V = p.sb("V", [128, NT, vw], BF16)
        if na:
            MT = p.sb("MT", [128, 4, 14, 256], BF16)
        with p.scope():
            _attn_proj(kind, W, QT, KT, V, vw)
        _attn_rest(kind, W, QT, KT, V, vw, MT if na else None)

    def _attn_proj(kind, W, QT, KT, V, vw):
        ga, na, sw = kind == "ga", kind == "na", kind == "swa"
        p.rot("xt", [p.sb("xt%d" % i, [128, 8, 512], F32) for i in range(2)])
        p.rot("ht", [p.sb("ht%d" % i, [128, 8, 512], BF16) for i in range(2)])
        if not na:
            p.rot("f1", [p.sb("f1_%d" % i, [128, 512], F32) for i in range(3)])
            p.rot("f2", [p.sb("f2_%d" % i, [128, 512], F32) for i in range(3)])
            p.rot("f3", [p.sb("f3_%d" % i, [128, 512], F32) for i in range(3)])
            p.rot("cs", [p.sb("cs_%d" % i, [128, 2, 512], F32) for i in range(2)])
        for (col0, C, is_ctx) in tok_chunks(512):
            ht, n = emit_load_h(p, nc, xTv, mod1p, mod, col0, C, is_ctx)
            if not na and not is_ctx:
                cs = p.nxt("cs")
                p.dma(cs[:, 0, 0:C], cos2[:, col0:col0 + C], w=[cs])
                p.dma(cs[:, 1, 0:C], sin2[:, col0:col0 + C], w=[cs])
            if na:
                for i in range(2):
                    ps = p.nxt("psJ")
                    emit_proj(p, ps, ps[:, 0:C], W, i * 128, 128, ht, n)
                    p.cp("act", QT[i], QT[i][:, col0:col0 + C], ps, ps[:, 0:C])
                    ps = p.nxt("psJ")
                    emit_proj(p, ps, ps[:, 0:C], W, 256 + i * 128, 128, ht, n)
                    p.cp("dve", KT[i], KT[i][:, col0:col0 + C], ps, ps[:, 0:C])
            else:
                items = [(QT[0], 0, 256, 0), (QT[1], 128, 384, 0), (KT[0], 512, 640, 2)]
                for (dst, c_a, c_r, gi) in items:
                    psa = p.nxt("psJ")
                    emit_proj(p, psa, psa[:, 0:C], W, c_a, 128, ht, n)
                    if not is_ctx:
                        psr = p.nxt("psJ")
                        emit_proj(p, psr, psr[:, 0:C], W, c_r, 128, ht, n)
                    if ga:
                        sq = p.nxt("f1")
                        p.act(sq, sq[:, 0:C], psa, psa[:, 0:C], AF.Square)
                        pss = p.nxt("psJ")
                        p.mm(pss, pss[:, 0:C], cst, cst[:, 4, :], sq, sq[:, 0:C])
                        ln = p.nxt("f1")
                        p.act(ln, ln[:, 0:C], pss, pss[:, 0:C], AF.Ln, bias=RMS_EPS, scale=1.0 / 64)
                        rs = p.nxt("f1")
                        p.act(rs, rs[:, 0:C], ln, ln[:, 0:C], AF.Exp, scale=-0.5)
                        qn = p.nxt("f2")
                        p.tt("dve", qn, qn[:, 0:C], psa, psa[:, 0:C], rs, rs[:, 0:C], ALU.mult)
                        if is_ctx:
                            p.ts("dve", dst, dst[:, col0:col0 + C], qn, qn[:, 0:C], gn[:, gi:gi + 1], None, ALU.mult, extra_r=[gn])
                        else:
                            qr = p.nxt("f2")
                            p.tt("dve", qr, qr[:, 0:C], psr, psr[:, 0:C], rs, rs[:, 0:C], ALU.mult)
                            t1 = p.nxt("f3")
                            p.stt("dve", t1, t1[:, 0:C], qn, qn[:, 0:C], gn[:, gi:gi + 1], cs, cs[:, 0, 0:C], ALU.mult, ALU.mult, extra_r=[gn])
                            t2 = p.nxt("f3")
                            p.stt("pool", t2, t2[:, 0:C], qr, qr[:, 0:C], gn[:, gi + 1:gi + 2], cs, cs[:, 1, 0:C], ALU.mult, ALU.mult, extra_r=[gn])
                            p.tt("pool", dst, dst[:, col0:col0 + C], t1, t1[:, 0:C], t2, t2[:, 0:C], ALU.add)
                    else:
                        if is_ctx:
                            p.cp("act", dst, dst[:, col0:col0 + C], psa, psa[:, 0:C])
                        else:
                            t1 = p.nxt("f3")
                            p.tt("dve", t1, t1[:, 0:C], psa, psa[:, 0:C], cs, cs[:, 0, 0:C], ALU.mult)
                            t2 = p.nxt("f3")
                            p.tt("dve", t2, t2[:, 0:C], psr, psr[:, 0:C], cs, cs[:, 1, 0:C], ALU.mult)
                            p.tt("pool", dst, dst[:, col0:col0 + C], t1, t1[:, 0:C], t2, t2[:, 0:C], ALU.add)
            vc0 = 512 if na else 768
            for tt_ in range(C // 128):
                ps = p.nxt("psJ")
                for kc in range(8):
                    p.mm(ps, ps[:, 0:vw], ht, ht[:, kc, tt_ * 128:(tt_ + 1) * 128], W, W[:, kc, vc0:vc0 + vw],
                         start=(kc == 0), stop=(kc == 7))
                p.cp("act", V, V[:, col0 // 128 + tt_, :], ps, ps[:, 0:vw])

    def _attn_rest(kind, W, QT, KT, V, vw, MT):
        ga, na, sw = kind == "ga", kind == "na", kind == "swa"
        if na:
            with p.scope():
                mk = p.sb("mk", [128, 14, 256], F32)
                p.dma(mk[:], na_mask.rearrange("v k q -> k v q"), w=[mk])
                bst = [p.sb("bst%d" % i, [128, 14, 256], F32) for i in range(2)]
                for h in range(4):
                    b_ = bst[h % 2]
                    p.dma(b_[:], na_bias[h].rearrange("v k q -> k v q"), w=[b_])
                    p.act(b_, b_[:], b_, b_[:], AF.Exp)
                    p.tt("dve", MT, MT[:, h, :, :], b_, b_[:], mk, mk[:], ALU.mult)
        if sw:
            MS = p.sb("MS", [128, 4, 256], BF16)
            p.dma(MS[:], sw_mask.rearrange("v k q -> k v q"), w=[MS])
            sk = p.sb("sk", [128, 4], F32)
            p.dma(sk[:], sinkv, w=[sk])
            p.act(sk, sk[:], sk, sk[:], AF.Exp)
        CA = 512 if ga else 256
        p.rot("sbP", [p.sb("sbP%d" % i, [128, 512], BF16) for i in range(4)])
        p.rot("sbP2", [p.sb("sbP2%d" % i, [128, 512], BF16) for i in range(3)])
        p.rot("sbR", [p.sb("sbR%d" % i, [64, 512], F32) for i in range(2)])
        p.rot("tmpo", [p.sb("tmpo%d" % i, [64, 512], BF16) for i in range(2)])
        p.rot("ost", [p.sb("ost%d" % i, [128, 2, 512], BF16) for i in range(2)])
        slot = {"na": 0, "swa": 1, "ga": 2}[kind]
        for (col0, C, is_ctx) in tok_chunks(CA):
            ost = p.nxt("ost")
            for h in range(4):
                pr, odd = h // 2, h % 2
                rows = slice(odd * 64, odd * 64 + 64)
                ktb = KT[pr] if na else KT[0]
                vcol = slice(h * 64, h * 64 + 64) if na else slice(0, 64)
                blocks = []
                if not is_ctx:
                    if ga:
                        kts = [(kt, None, None) for kt in range(S_LAT // 128)]
                    elif sw:
                        qt0 = col0 // 128
                        kts = []
                        for j in range(4):
                            kt = qt0 - 1 + j
                            if 0 <= kt < S_LAT // 128:
                                kts.append((kt, MS, MS[:, j, :]))
                    else:
                        qc = col0 // 256
                        if qc == 0:
                            kts = [(j, MT, MT[:, h, 6 + j, :]) for j in range(4)]
                        elif qc == 31:
                            kts = [(60 + j, MT, MT[:, h, 10 + j, :]) for j in range(4)]
                        else:
                            kts = [(2 * qc - 2 + j, MT, MT[:, h, j, :]) for j in range(6)]
                    for (kt, Mb, m_ap) in kts:
                        blocks.append((ktb, ktb[rows, kt * 128:(kt + 1) * 128], V, V[:, kt, vcol], Mb, m_ap))
                for kt in range(S_LAT // 128, NT):
                    blocks.append((ktb, ktb[rows, kt * 128:(kt + 1) * 128], V, V[:, kt, vcol], None, None))
                extra = (sk, sk[0:64, h:h + 1]) if sw else None
                emit_attention(p, nc, QT[pr], QT[pr][rows, col0:col0 + C], C, blocks, onesb,
                               ost, ost[rows, pr, 0:C], odd, extra, "tmpo")
            for pr in range(2):
                p.dma(u_out[slot, pr, :, col0:col0 + C], ost[:, pr, 0:C], r=[ost])

    def ssd_phase():
        with p.scope():
            _ssd_phase()

    def _ssd_phase():
        W = p.sb("W", [128, 8, 776], BF16)
        p.dma(W[:], w_sd.rearrange("(kc p) n -> p kc n", p=128), w=[W], q="pool")
        sm = p.sb("sm", [128, 32], F32)
        p.dma(sm[:], ssd_small, w=[sm])
        cw = p.sb("cw", [128, 4, 5], F32)
        p.dma(cw[:], ssd_convw, w=[cw])
        A = p.sb("A", [128, 8], F32)
        p.act(A, A[:], sm, sm[:, 0:8], AF.Exp)
        p.ts("dve", A, A[:], A, A[:], -1.0, None, ALU.mult)
        XC = p.sb("XC", [128, 2, T], F32)
        BT = p.sb("BT", [128, T], BF16)
        CT = p.sb("CT", [128, T], BF16)
        SZ = p.sb("SZ", [128, 2, T], BF16)
        DT = p.sb("DT", [128, NT, 8], F32)
        DA = p.sb("DA", [128, NT, 8], F32)
        with p.scope():
            p.rot("xt", [p.sb("xt%d" % i, [128, 8, 260], F32) for i in range(2)])
            p.rot("ht", [p.sb("ht%d" % i, [128, 8, 260], BF16) for i in range(2)])
            p.rot("pre", [p.sb("pre%d" % i, [128, 260], F32) for i in range(3)])
            p.rot("acc", [p.sb("acc%d" % i, [128, 256], F32) for i in range(3)])
            p.rot("sp", [p.sb("sp%d" % i, [128, 8], F32) for i in range(6)])
            for (col0, C, is_ctx) in tok_chunks(256):
                s0, s1 = (S_LAT, T) if is_ctx else (0, S_LAT)
                lo = 2 if col0 - 2 >= s0 else 0
                hi = 2 if col0 + C + 2 <= s1 else 0
                ht, n = emit_load_h(p, nc, xTv, mod1p, mod, col0, C, is_ctx, lo, hi)
                for ci in range(4):
                    ps = p.nxt("psJ")
                    emit_proj(p, ps, ps[:, 0:n], W, ci * 128, 128, ht, n)
                    pre = p.nxt("pre")
                    if lo == 0:
                        p.memset("pool", pre, pre[:, 0:2], 0.0)
                    if hi == 0:
                        p.memset("pool", pre, pre[:, C + 2:C + 4], 0.0)
                    p.cp("act", pre, pre[:, 2 - lo:2 - lo + n], ps, ps[:, 0:n])
                    acc = p.nxt("acc")
                    e = "dve" if ci % 2 == 0 else "pool"
                    p.ts(e, acc, acc[:, 0:C], pre, pre[:, 0:C], cw[:, ci, 0:1], None, ALU.mult, extra_r=[cw])
                    for j in range(1, 5):
                        p.stt(e, acc, acc[:, 0:C], pre, pre[:, j:j + C], cw[:, ci, j:j + 1], acc, acc[:, 0:C], ALU.mult, ALU.add, extra_r=[cw])
                    if ci < 2:
                        dst, d_ap = XC, XC[:, ci, col0:col0 + C]
                    elif ci == 2:
                        dst, d_ap = BT, BT[:, col0:col0 + C]
                    else:
                        dst, d_ap = CT, CT[:, col0:col0 + C]
                    p.act(dst, d_ap, acc, acc[:, 0:C], AF.Silu, bias=sm[:, 16 + ci:17 + ci], extra_r=[sm])
                for i in range(2):
                    ps = p.nxt("psJ")
                    for kc in range(8):
                        p.mm(ps, ps[:, 0:C], W, W[:, kc, 512 + i * 128:640 + i * 128], ht, ht[:, kc, lo:lo + C],
                             start=(kc == 0), stop=(kc == 7))
                    p.act(SZ, SZ[:, i, col0:col0 + C], ps, ps[:, 0:C], AF.Silu)
                for tt_ in range(C // 128):
                    ti = col0 // 128 + tt_
                    ps = p.nxt("psJ")
                    for kc in range(8):
                        p.mm(ps, ps[:, 0:8], ht, ht[:, kc, lo + tt_ * 128:lo + (tt_ + 1) * 128], W, W[:, kc, 768:776],
                             start=(kc == 0), stop=(kc == 7))
                    xv = p.nxt("sp")
                    p.tt("dve", xv, xv[:], ps, ps[:, 0:8], sm, sm[:, 8:16], ALU.add)
                    ab = p.nxt("sp")
                    p.ts("dve", ab, ab[:], xv, xv[:], -1.0, None, ALU.mult)
                    p.tt("dve", ab, ab[:], ab, ab[:], xv, xv[:], ALU.max)
                    ex = p.nxt("sp")
                    p.act(ex, ex[:], ab, ab[:], AF.Exp, scale=-1.0)
                    p.act(ex, ex[:], ex, ex[:], AF.Ln, bias=1.0)
                    p.stt("dve", DT, DT[:, ti, :], xv, xv[:], 0.0, ex, ex[:], ALU.max, ALU.add)
                    p.tt("dve", DA, DA[:, ti, :], DT, DT[:, ti, :], A, A[:], ALU.mult)
        hst = [[p.sb("hst%d_%d" % (d, h), [128, 64], F32) for h in range(4)] for d in range(2)]
        hpad = [[p.sb("hpad%d_%d" % (d, h), [128, 128], BF16) for h in range(4)] for d in range(2)]
        for d in range(2):
            for h in range(4):
                p.memset("pool", hst[d][h], hst[d][h][:], 0.0)
                p.memset("pool", hpad[d][h], hpad[d][h][:], 0.0)
        xpad = [[p.sb("xpad%d_%d" % (o, i), [128, 128], BF16) for i in range(2)] for o in range(2)]
        for o in range(2):
            for i in range(2):
                p.memset("pool", xpad[o][i], xpad[o][i][:], 0.0)
        xpi = [0, 0]
        p.rot("xtok", [p.sb("xtok%d" % i, [128, 256], F32) for i in range(2)])
        p.rot("btok", [p.sb("btok%d" % i, [128, 128], BF16) for i in range(2)])
        p.rot("gm", [p.sb("gm%d" % i, [128, 128], F32) for i in range(2)])
        p.rot("actok", [p.sb("actok%d" % i, [128, 4], F32) for i in range(2)])
        p.rot("darep", [p.sb("darep%d" % i, [128, 128], F32) for i in range(3)])
        p.rot("g1", [p.sb("g1_%d" % i, [128, 128], F32) for i in range(4)])
        p.rot("g2", [p.sb("g2_%d" % i, [128, 128], F32) for i in range(4)])
        p.rot("b1", [p.sb("b1_%d" % i, [128, 128], BF16) for i in range(4)])
        p.rot("b2", [p.sb("b2_%d" % i, [128, 128], BF16) for i in range(4)])
        p.rot("xd", [p.sb("xd_%d" % i, [128, 64], BF16) for i in range(4)])
        p.rot("sc", [p.sb("sc_%d" % i, [128, 4], F32) for i in range(8)])
        p.rot("yst", [p.sb("yst%d" % i, [128, 256], F32) for i in range(3)])
        p.rot("yld", [p.sb("yld%d" % i, [128, 256], F32) for i in range(3)])
        p.rot("yo", [p.sb("yo%d" % i, [128, 256], F32) for i in range(3)])
        p.rot("qA", psb[0:2])
        p.rot("qB", psb[2:4])
        p.rot("qY", psb[4:6])
        p.rot("qS", psb[6:7])
        ysb = [Buf(None, "ysc%d" % i) for i in range(NT)]
        ident = cst[:, 0, :]
        import os as _os
        _lim = int(_os.environ.get("SSD_LIM", "1000"))
        for d in range(2):
            order = [64, 65] + list(range(64)) if d == 0 else [65, 64] + list(range(63, -1, -1))
            order = order[:_lim]
            tri = cst[:, 1, :] if d == 0 else cst[:, 2, :]
            edge = 127 if d == 0 else 0
            for c in order:
                cols = slice(c * 128, (c + 1) * 128)
                xtok = p.nxt("xtok")
                for i in range(2):
                    pt = p.nxt("qA")
                    p.tr(pt, pt[:, 0:128], XC, XC[:, i, cols], cst, ident)
                    p.cp("act", xtok, xtok[:, i * 128:(i + 1) * 128], pt, pt[:, 0:128])
                btok = p.nxt("btok")
                p.tr(psbf, psbf[:, 0:128], BT, BT[:, cols], identb, identb[:])
                p.cp("act", btok, btok[:], psbf, psbf[:, 0:128])
                pg = p.nxt("qA")
                p.mm(pg, pg[:, 0:128], BT, BT[:, cols], CT, CT[:, cols])
                gm = p.nxt("gm")
                p.tt("dve", gm, gm[:], pg, pg[:, 0:128], cst, tri, ALU.mult)
                pa = p.nxt("qA")
                p.mm(pa, pa[:, 0:4], cst, tri, DA, DA[:, c, d * 4:d * 4 + 4])
                actok = p.nxt("actok")
                p.cp("dve", actok, actok[:], pa, pa[:, 0:4])
                Y = p.nxt("qY")
                for h in range(4):
                    pr, odd = h // 2, h % 2
                    darep = p.nxt("darep")
                    p.ts("pool", darep, darep[:], cst, cst[:, 3, :], DA[:, c, d * 4 + h:d * 4 + h + 1], None, ALU.mult, extra_r=[DA])
                    pb = p.nxt("qB")
                    p.mm(pb, pb[:, 0:128], darep, darep[:], cst, tri)
                    sc = p.nxt("sc")
                    p.cp("dve", sc, sc[:, 0:1], pb, pb[:, edge:edge + 1])
                    diff = p.nxt("g1")
                    p.ts("dve", diff, diff[:], pb, pb[:, 0:128], actok[:, h:h + 1], 0.0, ALU.subtract, ALU.min, extra_r=[actok])
                    p.act(diff, diff[:], diff, diff[:], AF.Exp)
                    mt = p.nxt("b1")
                    p.tt("pool", mt, mt[:], diff, diff[:], gm, gm[:], ALU.mult)
                    e2 = p.nxt("g2")
                    p.act(e2, e2[:], pb, pb[:, 0:128], AF.Exp)
                    csb = p.nxt("b2")
                    p.tt("dve", csb, csb[:], CT, CT[:, cols], e2, e2[:], ALU.mult)
                    p.act(sc, sc[:, 1:2], sc, sc[:, 0:1], AF.Exp)
                    p.act(sc, sc[:, 2:3], actok, actok[:, h:h + 1], AF.Exp, bias=sc[:, 0:1], scale=-1.0, extra_r=[sc])
                    p.tt("dve", sc, sc[:, 3:4], sc, sc[:, 2:3], DT, DT[:, c, d * 4 + h:d * 4 + h + 1], ALU.mult)
                    xp = xpad[odd][xpi[odd] % 2]
                    xpi[odd] += 1
                    p.ts("dve", xp, xp[:, odd * 64:odd * 64 + 64], xtok, xtok[:, h * 64:h * 64 + 64],
                         DT[:, c, d * 4 + h:d * 4 + h + 1], None, ALU.mult, extra_r=[DT])
                    xd = p.nxt("xd")
                    p.ts("pool", xd, xd[:], xtok, xtok[:, h * 64:h * 64 + 64], sc[:, 3:4], None, ALU.mult, extra_r=[sc])
                    ycols = slice(pr * 128, pr * 128 + 128)
                    p.mm(Y, Y[:, ycols], xp, xp[:], mt, mt[:], start=(odd == 0), stop=False)
                    p.mm(Y, Y[:, ycols], hpad[d][h], hpad[d][h][:], csb, csb[:], start=False, stop=(odd == 1))
                    pst = p.nxt("qS")
                    p.mm(pst, pst[:, 0:64], btok, btok[:], xd, xd[:])
                    p.stt("dve", hst[d][h], hst[d][h][:], hst[d][h], hst[d][h][:], sc[:, 1:2], pst, pst[:, 0:64],
                          ALU.mult, ALU.add, extra_r=[sc])
                    p.cp("act", hpad[d][h], hpad[d][h][:, odd * 64:odd * 64 + 64], hst[d][h], hst[d][h][:])
                if d == 0:
                    yst = p.nxt("yst")
                    p.cp("act", yst, yst[:], Y, Y[:, 0:256])
                    p.dma(ysc[c], yst[:], r=[yst], w=[ysb[c]])
                else:
                    yld = p.nxt("yld")
                    p.dma(yld[:], ysc[c], r=[ysb[c]], w=[yld])
                    yo = p.nxt("yo")
                    for i in range(2):
                        sl = slice(i * 128, (i + 1) * 128)
                        p.stt("dve", yo, yo[:, sl], XC, XC[:, i, cols], sm[:, 20 + i:21 + i], Y, Y[:, sl], ALU.mult, ALU.add, extra_r=[sm])
                        p.tt("pool", yo, yo[:, sl], yo, yo[:, sl], yld, yld[:, sl], ALU.add)
                        p.tt("pool", yo, yo[:, sl], yo, yo[:, sl], SZ, SZ[:, i, cols], ALU.mult)
                        p.dma(yg_out[i, :, cols], yo[:, sl], r=[yo])

    for ph in phases:
        if ph == "ssd":
            ssd_phase()
        else:
            attn_phase(ph)
    p.finish()
    return nc


_CONST = {}


def _consts():
    if _CONST:
        return _CONST
    k = np.arange(128)
    ident = np.eye(128, dtype=np.float32)
    tri = (k[:, None] <= k[None, :]).astype(np.float32)
    triT = (k[:, None] >= k[None, :]).astype(np.float32)
    ones = np.ones((128, 128), np.float32)
    blk = ((k[:, None] // 64) == (k[None, :] // 64)).astype(np.float32)
    _CONST["consts"] = np.ascontiguousarray(np.stack([ident, tri, triT, ones, blk], axis=1))
    _CONST["identb"] = ident.astype(ml_dtypes.bfloat16)
    t = np.arange(S_LAT, dtype=np.int32)
    row = (t // 64).astype(np.float32)
    col = (t % 64).astype(np.float32)
    inv = (np.float32(10000.0) ** (-np.arange(16, dtype=np.float32) / np.float32(16))).astype(np.float32)
    ang = np.concatenate([row[:, None] * inv, col[:, None] * inv], axis=-1).astype(np.float32)
    cs, sn = np.cos(ang).T, np.sin(ang).T
    cos64 = np.concatenate([cs, cs], 0)
    sin64 = np.concatenate([-sn, sn], 0)
    _CONST["cos2"] = np.ascontiguousarray(np.concatenate([cos64, cos64], 0).astype(np.float32))
    _CONST["sin2"] = np.ascontiguousarray(np.concatenate([sin64, sin64], 0).astype(np.float32))
    var = [(8, 14 + j) for j in range(6)] + [(0, j) for j in range(4)] + [(31, 60 + j) for j in range(4)]
    msk = np.zeros((14, 128, 256), np.float32)
    idr = np.zeros((14, 128, 256), np.int64)
    idc = np.zeros((14, 128, 256), np.int64)
    kk = np.arange(128)[:, None]
    qq = np.arange(256)[None, :]
    for v, (qc, kt) in enumerate(var):
        kr, kc = 2 * kt + kk // 64, kk % 64
        r, c = 4 * qc + qq // 64, qq % 64
        rs = np.clip(r - 4, 0, 120)
        c0 = np.clip(c - 8, 0, 48)
        ok = (kr >= rs) & (kr < rs + 8) & (kc >= c0) & (kc < c0 + 16)
        msk[v] = ok
        idr[v] = np.where(ok, kr - r + 7, 0)
        idc[v] = np.where(ok, kc - c + 15, 0)
    _CONST["na_mask"] = msk
    _CONST["na_idr"], _CONST["na_idc"] = idr, idc
    j = np.arange(4)[:, None, None]
    kpos = 128 * (j - 1) + np.arange(128)[None, :, None]
    qpos = np.arange(256)[None, None, :]
    _CONST["sw_mask"] = (np.abs(kpos - qpos) <= 128).astype(ml_dtypes.bfloat16)
    return _CONST


def _rotperm(cols):
    cols = np.asarray(cols).reshape(-1, 64)
    return np.concatenate([cols[:, 32:], cols[:, :32]], axis=1).reshape(-1)


def prep_M(I, l, xfull, ctxfull, b, hh):
    C = _consts()
    w_in = I["w_in"][l]
    m = {}
    m["xT"] = np.ascontiguousarray(np.concatenate([xfull[b], ctxfull[b]], 0).T)
    m["cvec"] = np.ascontiguousarray(np.stack([I["c"][b].reshape(8, 128).T, I["c_ctx"].reshape(8, 128).T], axis=-1))
    m["wmod"] = np.ascontiguousarray(I["w_mod"][l][:, 0:2048])
    m["bmod"] = np.ascontiguousarray(I["b_mod"][l][0:2048].reshape(16, 128).T)

    def qkv(qb, kb, vb):
        q = np.arange(qb + 256 * hh, qb + 256 * hh + 256)
        k = np.arange(kb + 64 * hh, kb + 64 * hh + 64)
        v = np.arange(vb + 64 * hh, vb + 64 * hh + 64)
        cols = np.concatenate([q, _rotperm(q), k, k, _rotperm(k), _rotperm(k), v])
        return np.ascontiguousarray(w_in[:, cols])

    m["w_ga"] = qkv(2304, 2816, 2944)
    m["w_sw"] = qkv(1536, 2048, 2176)
    na = np.concatenate([np.arange(o + 256 * hh, o + 256 * hh + 256) for o in (0, 512, 1024)])
    m["w_na"] = np.ascontiguousarray(w_in[:, na])
    dtc = np.array([4608 + d * 8 + 4 * hh + h for d in range(2) for h in range(4)])
    sd = np.concatenate([np.arange(3584 + 256 * hh, 3584 + 256 * hh + 256), np.arange(4096 + 128 * hh, 4096 + 128 * hh + 128),
                         np.arange(4352 + 128 * hh, 4352 + 128 * hh + 128), np.arange(3072 + 256 * hh, 3072 + 256 * hh + 256), dtc])
    m["w_sd"] = np.ascontiguousarray(w_in[:, sd])
    m["cos2"], m["sin2"] = C["cos2"], C["sin2"]
    d = np.arange(128) % 64
    gq, gk = I["qk_gain_q"][l], I["qk_gain_k"][l]
    m["gains"] = np.ascontiguousarray(np.stack([gq[d], gq[(d + 32) % 64], gk[d], gk[(d + 32) % 64]], axis=1))
    m["consts"], m["identb"] = C["consts"], C["identb"]
    m["sinkv"] = np.ascontiguousarray(np.broadcast_to(I["swa_sink"][l][4 * hh:4 * hh + 4][None, :], (128, 4)))
    rpb = I["na_rpb"][l][4 * hh:4 * hh + 4]
    m["na_bias"] = np.ascontiguousarray(rpb[:, C["na_idr"], C["na_idc"]])
    m["na_mask"] = C["na_mask"]
    m["sw_mask"] = C["sw_mask"]
    sm = np.zeros((128, 32), np.float32)
    sm[:, 0:8] = I["a_log"][l][:, 4 * hh:4 * hh + 4].reshape(1, 8)
    sm[:, 8:16] = I["dt_bias"][l][:, 4 * hh:4 * hh + 4].reshape(1, 8)
    pch = np.arange(128)
    chans = [256 * hh + pch, 256 * hh + 128 + pch, 512 + 128 * hh + pch, 768 + 128 * hh + pch]
    cw = np.zeros((128, 4, 5), np.float32)
    for ci, ch in enumerate(chans):
        sm[:, 16 + ci] = I["conv_b"][l][ch]
        cw[:, ci, :] = I["conv_w"][l][:, ch].T
    for i in range(2):
        sm[:, 20 + i] = I["d_skip"][l][4 * hh + 2 * i + pch // 64]
    m["ssd_small"] = sm
    m["ssd_convw"] = cw
    return m


TF = 4096 + 128


def f_chunks():
    return [(i * 512, 512, False) for i in range(8)] + [(4096, 128, True)]


def build_F(moe):
    nc = new_nc()
    p = P(nc)
    ff = D_FFE if moe else D_FF
    nj = ff // 128
    nexp = NE if moe else 1
    xT = din(nc, "xT", [D, TF])
    uT = din(nc, "uT", [3, 4, 128, TF], BF16)
    ygT = din(nc, "ygT", [4, 128, TF])
    cvec = din(nc, "cvec", [128, 8, 2])
    wmod = din(nc, "wmod", [D, 6144])
    bmod = din(nc, "bmod", [128, 48])
    w_gate = din(nc, "w_gate", [4, D, D])
    w_branch = din(nc, "w_branch", [4, 512, D])
    w_out = din(nc, "w_out", [D, D])
    vecs = din(nc, "vecs", [128, 72])
    consts = din(nc, "consts", [128, 5, 128])
    w_up = din(nc, "w_up", [nexp, D, 2 * ff])
    w_down = din(nc, "w_down", [nexp, ff, D])
    if moe:
        w_r = din(nc, "w_r", [128, 8, 8])
        b_r = din(nc, "b_r", [128, 8])
        selc = din(nc, "selc", [8, 8, 128])
    x_out = dout(nc, "x_out", [D, TF])

    xTv = xT.rearrange("(kc p) t -> p kc t", p=128)
    xov = x_out.rearrange("(kc p) t -> p kc t", p=128)
    psb = [p.ps("psb%d" % i, [128, 512], F32) for i in range(8)]
    p.rot("psJ", psb)

    cst = p.sb("cst", [128, 5, 128], F32)
    p.dma(cst[:], consts, w=[cst])
    ones = cst[:, 3, :]
    vc = p.sb("vc", [128, 72], F32)
    p.dma(vc[:], vecs, w=[vc])
    if moe:
        wr = p.sb("wr", [128, 8, 8], F32)
        p.dma(wr[:], w_r, w=[wr])
        br = p.sb("br", [128, 8], F32)
        p.dma(br[:], b_r, w=[br])
        sel = p.sb("sel", [8, 8, 128], F32)
        p.dma(sel[:], selc, w=[sel])
    mod1p = p.sb("mod1p", [128, 48, 2], F32)
    mod = p.sb("mod", [128, 48, 2], F32)
    with p.scope():
        mod_t = emit_modulation(p, nc, cvec, wmod, bmod, 48, psb, "mf")
        p.cp("dve", mod, mod[:], mod_t, mod_t[:])
        p.ts("dve", mod1p, mod1p[:], mod_t, mod_t[:], 1.0, None, ALU.add)

    p.rot("xa", [p.sb("xa%d" % i, [128, 8, 512], F32) for i in range(1)])
    p.rot("st", [p.sb("st%d" % i, [128, 512], F32) for i in range(6)])

    def layer_norm(xa, C, sq, gcol, bcol):
        psm = p.nxt("psJ")
        for kc in range(8):
            p.mm(psm, psm[:, 0:C], cst, ones, xa, xa[:, kc, 0:C], start=(kc == 0), stop=(kc == 7))
        for kc in range(8):
            p.act(sq, sq[:, kc, 0:C], xa, xa[:, kc, 0:C], AF.Square)
        pss = p.nxt("psJ")
        for kc in range(8):
            p.mm(pss, pss[:, 0:C], cst, ones, sq, sq[:, kc, 0:C], start=(kc == 0), stop=(kc == 7))
        mean = p.nxt("st")
        p.ts("dve", mean, mean[:, 0:C], psm, psm[:, 0:C], 1.0 / D, None, ALU.mult)
        m2 = p.nxt("st")
        p.tt("dve", m2, m2[:, 0:C], mean, mean[:, 0:C], mean, mean[:, 0:C], ALU.mult)
        var = p.nxt("st")
        p.stt("dve", var, var[:, 0:C], pss, pss[:, 0:C], 1.0 / D, m2, m2[:, 0:C], ALU.mult, ALU.subtract)
        p.act(var, var[:, 0:C], var, var[:, 0:C], AF.Ln, bias=LN_EPS)
        rstd = p.nxt("st")
        p.act(rstd, rstd[:, 0:C], var, var[:, 0:C], AF.Exp, scale=-0.5)
        for kc in range(8):
            p.tt("dve", xa, xa[:, kc, 0:C], xa, xa[:, kc, 0:C], mean, mean[:, 0:C], ALU.subtract)
            p.tt("pool", xa, xa[:, kc, 0:C], xa, xa[:, kc, 0:C], rstd, rstd[:, 0:C], ALU.mult)
            p.ts("dve", xa, xa[:, kc, 0:C], xa, xa[:, kc, 0:C], vc[:, gcol + kc:gcol + kc + 1], vc[:, bcol + kc:bcol + kc + 1],
                 ALU.mult, ALU.add, extra_r=[vc])

    for (col0, C, is_ctx) in f_chunks():
        w = 1 if is_ctx else 0
        xa = p.nxt("xa")
        with p.scope():
            xt = p.sb("xt", [128, 8, 512], F32)
            ht = p.sb("ht", [128, 8, 512], BF16)
            yg = p.sb("yg", [128, 4, 512], F32)
            ud = p.sb("ud", [128, 4, 512], BF16)
            sq = p.sb("sq", [128, 8, 512], F32)
            Wg = p.sb("Wg", [128, 8, 1024], BF16)
            p.rot("Wb", [p.sb("Wb%d" % i, [128, 4, 1024], BF16) for i in range(2)])
            p.rot("ub", [p.sb("ub%d" % i, [128, 4, 512], BF16) for i in range(2)])
            Wo = p.sb("Wo", [128, 8, 1024], BF16)
            mg = p.sb("mg", [128, 8, 512], F32)
            mgb = p.sb("mgb", [128, 8, 512], BF16)
            p.rot("tm", [p.sb("tm%d" % i, [128, 512], F32) for i in range(4)])
            p.dma(xt[:, :, 0:C], xTv[:, :, col0:col0 + C], w=[xt])
            p.dma(yg[:, :, 0:C], ygT.rearrange("k p t -> p k t")[:, :, col0:col0 + C], w=[yg])
            for kc in range(8):
                p.ts("dve", ht, ht[:, kc, 0:C], xt, xt[:, kc, 0:C], mod1p[:, 8 + kc, w:w + 1], mod[:, kc, w:w + 1], ALU.mult, ALU.add,
                     extra_r=[mod1p, mod])
            for k in range(4):
                p.act(sq, sq[:, k, 0:C], yg, yg[:, k, 0:C], AF.Square)
            pss = p.nxt("psJ")
            for k in range(4):
                p.mm(pss, pss[:, 0:C], cst, ones, sq, sq[:, k, 0:C], start=(k == 0), stop=(k == 3))
            rs = p.nxt("st")
            p.act(rs, rs[:, 0:C], pss, pss[:, 0:C], AF.Ln, bias=RMS_EPS, scale=1.0 / 512)
            p.act(rs, rs[:, 0:C], rs, rs[:, 0:C], AF.Exp, scale=-0.5)
            for k in range(4):
                p.stt("dve", ud, ud[:, k, 0:C], yg, yg[:, k, 0:C], vc[:, 64 + k:65 + k], rs, rs[:, 0:C], ALU.mult, ALU.mult, extra_r=[vc])
            for i in range(4):
                p.dma(Wg[:], w_gate[i].rearrange("(kc p) n -> p kc n", p=128), w=[Wg], q="pool")
                Wb = p.nxt("Wb")
                p.dma(Wb[:], w_branch[i].rearrange("(kc p) n -> p kc n", p=128), w=[Wb], q="pool")
                if i < 3:
                    ub = p.nxt("ub")
                    p.dma(ub[:, :, 0:C], uT[i].rearrange("k p t -> p k t")[:, :, col0:col0 + C], w=[ub])
                else:
                    ub = ud
                for o in range(8):
                    oc = slice(o * 128, (o + 1) * 128)
                    pg = p.nxt("psJ")
                    for kc in range(8):
                        p.mm(pg, pg[:, 0:C], Wg, Wg[:, kc, oc], ht, ht[:, kc, 0:C], start=(kc == 0), stop=(kc == 7))
                    pb = p.nxt("psJ")
                    for kc in range(4):
                        p.mm(pb, pb[:, 0:C], Wb, Wb[:, kc, oc], ub, ub[:, kc, 0:C], start=(kc == 0), stop=(kc == 3))
                    sg = p.nxt("tm")
                    p.act(sg, sg[:, 0:C], pg, pg[:, 0:C], AF.Sigmoid, bias=vc[:, i * 8 + o:i * 8 + o + 1], extra_r=[vc])
                    if i == 0:
                        p.tt("dve", mg, mg[:, o, 0:C], pb, pb[:, 0:C], sg, sg[:, 0:C], ALU.mult)
                    else:
                        t_ = p.nxt("tm")
                        p.tt("dve", t_, t_[:, 0:C], pb, pb[:, 0:C], sg, sg[:, 0:C], ALU.mult)
                        if i < 3:
                            p.tt("pool", mg, mg[:, o, 0:C], mg, mg[:, o, 0:C], t_, t_[:, 0:C], ALU.add)
                        else:
                            p.tt("pool", mgb, mgb[:, o, 0:C], mg, mg[:, o, 0:C], t_, t_[:, 0:C], ALU.add)
            p.dma(Wo[:], w_out.rearrange("(kc p) n -> p kc n", p=128), w=[Wo], q="pool")
            for o in range(8):
                oc = slice(o * 128, (o + 1) * 128)
                po = p.nxt("psJ")
                for kc in range(8):
                    p.mm(po, po[:, 0:C], Wo, Wo[:, kc, oc], mgb, mgb[:, kc, 0:C], start=(kc == 0), stop=(kc == 7))
                t_ = p.nxt("tm")
                p.ts("dve", t_, t_[:, 0:C], po, po[:, 0:C], mod[:, 16 + o, w:w + 1], None, ALU.mult, extra_r=[mod])
                p.stt("dve", xa, xa[:, o, 0:C], xt, xt[:, o, 0:C], float(ALPHA), t_, t_[:, 0:C], ALU.mult, ALU.add)
            layer_norm(xa, C, sq, 32, 40)
        with p.scope():
            h2 = p.sb("h2", [128, 8, 512], BF16)
            sq = p.sb("sq", [128, 8, 512], F32)
            a = p.sb("a", [128, nj, 512], BF16)
            p.rot("Wu", [p.sb("Wu%d" % i, [128, 8, 2, 256], BF16) for i in range(3)])
            p.rot("Wd", [p.sb("Wd%d" % i, [128, 4, 1024], BF16) for i in range(2)])
            p.rot("tm", [p.sb("tm%d" % i, [128, 512], F32) for i in range(4)])
            for kc in range(8):
                p.ts("dve", h2, h2[:, kc, 0:C], xa, xa[:, kc, 0:C], mod1p[:, 32 + kc, w:w + 1], mod[:, 24 + kc, w:w + 1], ALU.mult, ALU.add,
                     extra_r=[mod1p, mod])
            if moe:
                gbc = p.sb("gbc", [128, 8, 512], F32)
                facc = p.sb("facc", [128, 8, 512], F32)
                gT = p.sb("gT", [8, 512], F32)
                p.rot("r8", [p.sb("r8_%d" % i, [128, 8], F32) for i in range(8)])
                p.rot("r1", [p.sb("r1_%d" % i, [128, 1], F32) for i in range(8)])
                h2f = sq
                for kc in range(8):
                    p.ts("pool", h2f, h2f[:, kc, 0:C], xa, xa[:, kc, 0:C], mod1p[:, 32 + kc, w:w + 1], mod[:, 24 + kc, w:w + 1], ALU.mult, ALU.add,
                         extra_r=[mod1p, mod])
                for tt_ in range(C // 128):
                    tc_ = slice(tt_ * 128, (tt_ + 1) * 128)
                    pl = p.nxt("psJ")
                    for kc in range(8):
                        p.mm(pl, pl[:, 0:8], h2f, h2f[:, kc, tc_], wr, wr[:, kc, :], start=(kc == 0), stop=(kc == 7))
                    lg = p.nxt("r8")
                    p.tt("dve", lg, lg[:], pl, pl[:, 0:8], br, br[:], ALU.add)
                    m1 = p.nxt("r1")
                    p.op("dve", lambda: nc.vector.reduce_max(out=m1[:], in_=lg[:], axis=mybir.AxisListType.X), r=[lg], w=[m1])
                    eq = p.nxt("r8")
                    p.ts("dve", eq, eq[:], lg, lg[:], m1[:, 0:1], None, ALU.is_ge, extra_r=[m1])
                    l2 = p.nxt("r8")
                    p.stt("dve", l2, l2[:], eq, eq[:], -1e30, lg, lg[:], ALU.mult, ALU.add)
                    m2_ = p.nxt("r1")
                    p.op("dve", lambda: nc.vector.reduce_max(out=m2_[:], in_=l2[:], axis=mybir.AxisListType.X), r=[l2], w=[m2_])
                    s2 = p.nxt("r8")
                    p.ts("dve", s2, s2[:], lg, lg[:], m2_[:, 0:1], None, ALU.is_ge, extra_r=[m2_])
                    nm = p.nxt("r1")
                    p.ts("dve", nm, nm[:], m1, m1[:], -1.0, None, ALU.mult)
                    ex = p.nxt("r8")
                    p.act(ex, ex[:], lg, lg[:], AF.Exp, bias=nm[:, 0:1], extra_r=[nm])
                    p.tt("dve", ex, ex[:], ex, ex[:], s2, s2[:], ALU.mult)
                    dn = p.nxt("r1")
                    p.op("dve", lambda: nc.vector.reduce_sum(out=dn[:], in_=ex[:], axis=mybir.AxisListType.X), r=[ex], w=[dn])
                    p.op("dve", lambda: nc.vector.reciprocal(out=dn[:], in_=dn[:]), r=[dn], w=[dn])
                    p.ts("dve", ex, ex[:], ex, ex[:], dn[:, 0:1], None, ALU.mult, extra_r=[dn])
                    pt_ = p.nxt("psJ")
                    p.tr(pt_, pt_[0:8, 0:128], ex, ex[:], cst, cst[:, 0, :])
                    p.cp("act", gT, gT[:, tc_], pt_, pt_[0:8, 0:128])
                for e in range(NE):
                    pbc = p.nxt("psJ")
                    p.mm(pbc, pbc[:, 0:C], sel, sel[:, e, :], gT, gT[:, 0:C])
                    p.cp("act", gbc, gbc[:, e, 0:C], pbc, pbc[:, 0:C])
            for e in range(nexp):
                wuv = w_up[e].rearrange("(kc p) n -> p kc n", p=128)
                wdv = w_down[e].rearrange("(j p) n -> p j n", p=128)
                for j0 in range(0, nj, 2):
                    Wu = p.nxt("Wu")
                    p.dma(Wu[:, :, 0, :], wuv[:, :, j0 * 128:j0 * 128 + 256], w=[Wu], q="pool")
                    p.dma(Wu[:, :, 1, :], wuv[:, :, ff + j0 * 128:ff + j0 * 128 + 256], w=[Wu], q="pool")
                    for jj in range(2):
                        j = j0 + jj
                        jc = slice(jj * 128, (jj + 1) * 128)
                        pg = p.nxt("psJ")
                        for kc in range(8):
                            p.mm(pg, pg[:, 0:C], Wu, Wu[:, kc, 0, jc], h2, h2[:, kc, 0:C], start=(kc == 0), stop=(kc == 7))
                        pu = p.nxt("psJ")
                        for kc in range(8):
                            p.mm(pu, pu[:, 0:C], Wu, Wu[:, kc, 1, jc], h2, h2[:, kc, 0:C], start=(kc == 0), stop=(kc == 7))
                        sg = p.nxt("tm")
                        p.act(sg, sg[:, 0:C], pg, pg[:, 0:C], AF.Silu)
                        p.tt("dve", a, a[:, j, 0:C], pu, pu[:, 0:C], sg, sg[:, 0:C], ALU.mult)
                for j0 in range(0, nj, 4):
                    njj = min(4, nj - j0)
                    Wd = p.nxt("Wd")
                    p.dma(Wd[:, 0:njj, :], wdv[:, j0:j0 + njj, :], w=[Wd], q="pool")
                    for jj in range(njj):
                        j = j0 + jj
                        for o in range(8):
                            p.mm(psb[o], psb[o][:, 0:C], Wd, Wd[:, jj, o * 128:(o + 1) * 128], a, a[:, j, 0:C],
                                 start=(j == 0), stop=(j == nj - 1))
                for o in range(8):
                    if not moe:
                        t_ = p.nxt("tm")
                        p.ts("dve", t_, t_[:, 0:C], psb[o], psb[o][:, 0:C], mod[:, 40 + o, w:w + 1], None, ALU.mult, extra_r=[mod])
                        p.stt("dve", xa, xa[:, o, 0:C], xa, xa[:, o, 0:C], float(ALPHA), t_, t_[:, 0:C], ALU.mult, ALU.add)
                    elif e == 0:
                        p.tt("dve", facc, facc[:, o, 0:C], psb[o], psb[o][:, 0:C], gbc, gbc[:, e, 0:C], ALU.mult)
                    else:
                        t_ = p.nxt("tm")
                        p.tt("dve", t_, t_[:, 0:C], psb[o], psb[o][:, 0:C], gbc, gbc[:, e, 0:C], ALU.mult)
                        p.tt("pool", facc, facc[:, o, 0:C], facc, facc[:, o, 0:C], t_, t_[:, 0:C], ALU.add)
            if moe:
                for o in range(8):
                    t_ = p.nxt("tm")
                    p.ts("dve", t_, t_[:, 0:C], facc, facc[:, o, 0:C], mod[:, 40 + o, w:w + 1], None, ALU.mult, extra_r=[mod])
                    p.stt("dve", xa, xa[:, o, 0:C], xa, xa[:, o, 0:C], float(ALPHA), t_, t_[:, 0:C], ALU.mult, ALU.add)
            layer_norm(xa, C, sq, 48, 56)
            p.dma(xov[:, :, col0:col0 + C], xa[:, :, 0:C], r=[xa])
    p.finish()
    return nc


def prep_F(I, l, xfull, ctxfull, b, hh, u_pair, yg_pair):
    C = _consts()
    m = {}
    lat = slice(hh * 4096, (hh + 1) * 4096)
    cx = slice(hh * 128, (hh + 1) * 128)
    m["xT"] = np.ascontiguousarray(np.concatenate([xfull[b][lat], ctxfull[b][cx]], 0).T)
    tcols = np.concatenate([np.arange(hh * 4096, (hh + 1) * 4096), S_LAT + np.arange(hh * 128, (hh + 1) * 128)])
    u = np.concatenate([u_pair[0], u_pair[1]], axis=1)
    m["uT"] = np.ascontiguousarray(u[:, :, :, tcols])
    yg = np.concatenate([yg_pair[0], yg_pair[1]], axis=0)
    m["ygT"] = np.ascontiguousarray(yg[:, :, tcols])
    m["cvec"] = np.ascontiguousarray(np.stack([I["c"][b].reshape(8, 128).T, I["c_ctx"].reshape(8, 128).T], axis=-1))
    m["wmod"] = I["w_mod"][l]
    m["bmod"] = np.ascontiguousarray(I["b_mod"][l].reshape(48, 128).T)
    m["w_gate"] = I["w_gate"][l]
    m["w_branch"] = I["w_branch"][l]
    m["w_out"] = I["w_out"][l]
    vecs = np.zeros((128, 72), np.float32)
    vecs[:, 0:32] = I["b_gate"][l].reshape(4, 8, 128).transpose(2, 0, 1).reshape(128, 32)
    vecs[:, 32:40] = I["ln1_g"][l].reshape(8, 128).T
    vecs[:, 40:48] = I["ln1_b"][l].reshape(8, 128).T
    vecs[:, 48:56] = I["ln2_g"][l].reshape(8, 128).T
    vecs[:, 56:64] = I["ln2_b"][l].reshape(8, 128).T
    vecs[:, 64:68] = I["ssm_norm_g"][l].reshape(4, 128).T
    m["vecs"] = vecs
    m["consts"] = C["consts"]
    i = l // 2
    if l % 2 == 0:
        m["w_up"] = I["ffn_w_up"][i][None]
        m["w_down"] = I["ffn_w_down"][i][None]
    else:
        m["w_up"] = I["moe_w_up"][i]
        m["w_down"] = I["moe_w_down"][i]
        m["w_r"] = np.ascontiguousarray(I["moe_w_router"][i].reshape(8, 128, 8).transpose(1, 0, 2))
        m["b_r"] = np.ascontiguousarray(np.broadcast_to(I["moe_b_router"][i][None, :], (128, 8)))
        m["selc"] = np.ascontiguousarray(np.broadcast_to(np.eye(8, dtype=np.float32)[:, :, None], (8, 8, 128)))
    return m


_PROG = {}


def _prog(key):
    if key not in _PROG:
        _PROG[key] = build_M() if key == "M" else build_F(key == "Fmoe")
    return _PROG[key]


def kernel(**inputs):
    I = {k: np.asarray(v) for k, v in inputs.items()}
    x = np.ascontiguousarray(I["x"], dtype=np.float32)
    ctx = np.ascontiguousarray(I["ctx"], dtype=np.float32)
    cores = [(b, hh) for b in range(4) for hh in range(2)]
    for l in range(DEPTH):
        maps = [prep_M(I, l, x, ctx, b, hh) for (b, hh) in cores]
        res = run_bass_kernel_spmd(_prog("M"), maps, core_ids=list(range(8))).results
        maps = []
        for ci, (b, hh) in enumerate(cores):
            up = [res[2 * b]["u_out"], res[2 * b + 1]["u_out"]]
            yp = [res[2 * b]["yg_out"], res[2 * b + 1]["yg_out"]]
            maps.append(prep_F(I, l, x, ctx, b, hh, up, yp))
        res = run_bass_kernel_spmd(_prog("Fmoe" if l % 2 else "Fdense"), maps, core_ids=list(range(8))).results
        xn = np.empty_like(x)
        cn = np.empty_like(ctx)
        for ci, (b, hh) in enumerate(cores):
            o = res[ci]["x_out"]
            xn[b, hh * 4096:(hh + 1) * 4096] = o[:, 0:4096].T
            cn[b, hh * 128:(hh + 1) * 128] = o[:, 4096:].T
        x, ctx = xn, cn
    return x
```
